# Optimizing a Trainium2 kernel written in Bass

```python
import math
import jax, jax.numpy as jnp
from jax import lax
import numpy as np

D_MODEL = 1024
BATCH = 2
SEQ = 8192
DEPTH = 4

N_A_LAYERS = DEPTH // 2
N_B_LAYERS = DEPTH - N_A_LAYERS
BLOCK = 128
EPS = 1e-6
ROPE_THETA = 10000.0

A_HEADS = 16
A_HEAD_DIM = D_MODEL // A_HEADS

B_HEAD_DIM = 128
B_HEADS = D_MODEL // B_HEAD_DIM
B_WINDOWS = (128, 512, 2048)
B_DILATIONS = (1, 4, 16)
B_GROUPS = len(B_WINDOWS)

MOE_GROUPS = 4
MOE_EXPERTS_PER_GROUP = 4
MOE_EXPERTS = MOE_GROUPS * MOE_EXPERTS_PER_GROUP
MOE_TOPK = 2
MOE_FF = D_MODEL // 4

kernel_name = "yoco_fox_dilated_hmoe_trunk"


def rms_norm(x, gain):
    xf = x.astype(jnp.float32)
    y = xf * lax.rsqrt(jnp.mean(xf * xf, axis=-1, keepdims=True) + EPS)
    return (y * gain.astype(jnp.float32)).astype(x.dtype)


def rope(x, positions):
    half = x.shape[-1] // 2
    inv_freq = ROPE_THETA ** (-jnp.arange(half, dtype=jnp.float32) / half)
    ang = positions.astype(jnp.float32)[..., None] * inv_freq
    cos = jnp.cos(ang)[:, :, None, :]
    sin = jnp.sin(ang)[:, :, None, :]
    xf = x.astype(jnp.float32)
    x1, x2 = xf[..., :half], xf[..., half:]
    out = jnp.concatenate([x1 * cos - x2 * sin, x2 * cos + x1 * sin], axis=-1)
    return out.astype(x.dtype)


def forgetting_attention(h, w_in, b_f, q_gain, k_gain, w_out):
    Bsz, S, D = h.shape
    proj = h @ w_in
    q = proj[..., :D].reshape(Bsz, S, A_HEADS, A_HEAD_DIM)
    k = proj[..., D:2 * D].reshape(Bsz, S, A_HEADS, A_HEAD_DIM)
    v = proj[..., 2 * D:3 * D].reshape(Bsz, S, A_HEADS, A_HEAD_DIM)
    f_logit = proj[..., 3 * D:]
    q = rms_norm(q, q_gain)
    k = rms_norm(k, k_gain)
    log_f = jax.nn.log_sigmoid(f_logit.astype(jnp.float32) + b_f.astype(jnp.float32))
    c = jnp.cumsum(log_f, axis=1)
    c_k = c.transpose(0, 2, 1)
    v32 = v.astype(jnp.float32)
    scale = A_HEAD_DIM ** -0.5
    nb = S // BLOCK
    q_blk = q.reshape(Bsz, nb, BLOCK, A_HEADS, A_HEAD_DIM).transpose(1, 0, 2, 3, 4)
    c_blk = c.reshape(Bsz, nb, BLOCK, A_HEADS).transpose(1, 0, 2, 3)
    starts = jnp.arange(nb, dtype=jnp.int32) * BLOCK
    k_pos = jnp.arange(S, dtype=jnp.int32)

    def one_block(args):
        qb, cb, start = args
        s = jnp.einsum('bqhd,bshd->bhqs', qb, k, preferred_element_type=jnp.float32) * scale
        s = s + cb.transpose(0, 2, 1)[..., None] - c_k[:, :, None, :]
        q_pos = start + jnp.arange(BLOCK, dtype=jnp.int32)
        causal = k_pos[None, :] <= q_pos[:, None]
        s = jnp.where(causal, s, -jnp.inf)
        p = jax.nn.softmax(s, axis=-1)
        return jnp.einsum('bhqs,bshd->bqhd', p, v32).astype(h.dtype)

    o = lax.map(one_block, (q_blk, c_blk, starts))
    o = o.transpose(1, 0, 2, 3, 4).reshape(Bsz, S, D)
    return o @ w_out


def shared_kv(h_stream, kv_norm, kv_w, k_gain, positions):
    Bsz, S, _ = h_stream.shape
    kv = rms_norm(h_stream, kv_norm) @ kv_w
    width = B_HEADS * B_HEAD_DIM
    k = kv[..., :width].reshape(Bsz, S, B_HEADS, B_HEAD_DIM)
    v = kv[..., width:].reshape(Bsz, S, B_HEADS, B_HEAD_DIM)
    k = rope(rms_norm(k, k_gain), positions)
    return k, v


def dilated_group(q, k, v, window, dilation):
    Bsz, S, H, Dh = q.shape
    band = window // dilation
    span = dilation * BLOCK
    s_pad = -(-S // span) * span
    L = s_pad // dilation
    nb = L // BLOCK

    def strided(t):
        X = t.shape[-1]
        t = jnp.pad(t, ((0, 0), (0, s_pad - S), (0, 0), (0, 0)))
        t = t.reshape(Bsz, L, dilation, H, X).transpose(0, 2, 1, 3, 4)
        return t.reshape(Bsz, dilation, nb, BLOCK, H, X)

    def with_prev(t):
        prev = jnp.pad(t, ((0, 0), (0, 0), (1, 0), (0, 0), (0, 0), (0, 0)))[:, :, :-1]
        return jnp.concatenate([prev, t], axis=3)

    qs = strided(q)
    kb = with_prev(strided(k))
    vb = with_prev(strided(v)).astype(jnp.float32)
    s = jnp.einsum('brnqhd,brnkhd->brnhqk', qs, kb,
                   preferred_element_type=jnp.float32) * (Dh ** -0.5)
    qi = jnp.arange(BLOCK)[:, None]
    kk = jnp.arange(2 * BLOCK)[None, :]
    dist = BLOCK + qi - kk
    in_band = (dist >= 0) & (dist <= band)
    before_start = (jnp.arange(nb)[:, None, None] == 0) & (kk[None] < BLOCK)
    valid = in_band[None] & ~before_start
    s = jnp.where(valid[None, None, :, None, :, :], s, -jnp.inf)
    m = jnp.max(s, axis=-1, keepdims=True)
    p = jnp.exp(s - m)
    l = jnp.sum(p, axis=-1, keepdims=True)
    o = jnp.einsum('brnhqk,brnkhd->brnqhd', p, vb) / l.transpose(0, 1, 2, 4, 3, 5)
    lse = (m + jnp.log(l))[..., 0].transpose(0, 1, 2, 4, 3)

    def unstride(t):
        X = t.shape[-1]
        t = t.reshape(Bsz, dilation, L, H, X).transpose(0, 2, 1, 3, 4)
        return t.reshape(Bsz, s_pad, H, X)[:, :S]

    return unstride(o), unstride(lse[..., None])[..., 0]


def dilated_attention(h, positions, k, v, w_q, q_gain, w_out):
    Bsz, S, D = h.shape
    q = (h @ w_q).reshape(Bsz, S, B_GROUPS, B_HEADS, B_HEAD_DIM)
    outs, lses = [], []
    for g in range(B_GROUPS):
        qg = rope(rms_norm(q[:, :, g], q_gain[g]), positions)
        o_g, lse_g = dilated_group(qg, k, v, B_WINDOWS[g], B_DILATIONS[g])
        outs.append(o_g)
        lses.append(lse_g)
    wgt = jax.nn.softmax(jnp.stack(lses, axis=0), axis=0)
    o = jnp.sum(wgt[..., None] * jnp.stack(outs, axis=0), axis=0)
    return o.reshape(Bsz, S, B_HEADS * B_HEAD_DIM).astype(h.dtype) @ w_out


def hier_moe(h, group_w, group_b, expert_w, expert_b, w_gate, w_up, w_down):
    Bsz, S, D = h.shape
    xt = h.reshape(Bsz * S, D)
    g_prob = jax.nn.softmax((xt @ group_w).astype(jnp.float32) + group_b.astype(jnp.float32), axis=-1)
    g_p, g_idx = lax.top_k(g_prob, 1)
    e_logits = ((xt @ expert_w).astype(jnp.float32) + expert_b.astype(jnp.float32))
    e_logits = e_logits.reshape(-1, MOE_GROUPS, MOE_EXPERTS_PER_GROUP)
    in_group = jnp.take_along_axis(e_logits, g_idx[:, :, None], axis=1)[:, 0]
    top_v, top_i = lax.top_k(in_group, MOE_TOPK)
    gate = g_p * jax.nn.softmax(top_v, axis=-1)
    eid = g_idx * MOE_EXPERTS_PER_GROUP + top_i
    comb = jnp.sum(jax.nn.one_hot(eid, MOE_EXPERTS, dtype=jnp.float32) * gate[..., None], axis=1)
    hid = jax.nn.silu(jnp.einsum('td,edf->tef', xt, w_gate)) * jnp.einsum('td,edf->tef', xt, w_up)
    y = jnp.einsum('tef,efd->td', hid * comb[..., None].astype(hid.dtype), w_down)
    return y.reshape(Bsz, S, D)


def setup_inputs(seed: int = 0) -> dict:
    key = jax.random.key(seed)
    ks = jax.random.split(key, 24)
    f32 = jnp.float32
    D = D_MODEL
    res_scale = (2.0 * DEPTH) ** -0.5
    bw = B_HEADS * B_HEAD_DIM

    def nrm(k, shape, scale):
        return jax.random.normal(k, shape, f32) * scale

    def gain(k, shape):
        return 1.0 + 0.02 * jax.random.normal(k, shape, f32)

    x = jax.random.normal(ks[0], (BATCH, SEQ, D), f32)
    positions = jnp.broadcast_to(jnp.arange(SEQ, dtype=jnp.int32), (BATCH, SEQ))
    return {
        "x": x,
        "positions": positions,
        "a_norm": gain(ks[1], (N_A_LAYERS, D)),
        "a_w_in": nrm(ks[2], (N_A_LAYERS, D, 3 * D + A_HEADS), D ** -0.5),
        "a_b_f": jax.random.uniform(ks[3], (N_A_LAYERS, A_HEADS), f32, 1.0, 5.0),
        "a_q_gain": gain(ks[4], (N_A_LAYERS, A_HEAD_DIM)),
        "a_k_gain": gain(ks[5], (N_A_LAYERS, A_HEAD_DIM)),
        "a_w_out": nrm(ks[6], (N_A_LAYERS, D, D), D ** -0.5 * res_scale),
        "kv_norm": gain(ks[7], (D,)),
        "kv_w": nrm(ks[8], (D, 2 * bw), D ** -0.5),
        "kv_k_gain": gain(ks[9], (B_HEAD_DIM,)),
        "b_norm": gain(ks[10], (N_B_LAYERS, D)),
        "b_w_q": nrm(ks[11], (N_B_LAYERS, D, B_GROUPS * bw), D ** -0.5),
        "b_q_gain": gain(ks[12], (N_B_LAYERS, B_GROUPS, B_HEAD_DIM)),
        "b_w_out": nrm(ks[13], (N_B_LAYERS, bw, D), bw ** -0.5 * res_scale),
        "ffn_norm": gain(ks[14], (DEPTH, D)),
        "moe_group_w": nrm(ks[15], (DEPTH, D, MOE_GROUPS), D ** -0.5),
        "moe_group_b": nrm(ks[16], (DEPTH, MOE_GROUPS), 0.01),
        "moe_expert_w": nrm(ks[17], (DEPTH, D, MOE_EXPERTS), D ** -0.5),
        "moe_expert_b": nrm(ks[18], (DEPTH, MOE_EXPERTS), 0.01),
        "moe_w_gate": nrm(ks[19], (DEPTH, MOE_EXPERTS, D, MOE_FF), D ** -0.5),
        "moe_w_up": nrm(ks[20], (DEPTH, MOE_EXPERTS, D, MOE_FF), D ** -0.5),
        "moe_w_down": nrm(ks[21], (DEPTH, MOE_EXPERTS, MOE_FF, D), MOE_FF ** -0.5 * res_scale),
    }


def reference(x, positions, a_norm, a_w_in, a_b_f, a_q_gain, a_k_gain, a_w_out,
              kv_norm, kv_w, kv_k_gain, b_norm, b_w_q, b_q_gain, b_w_out,
              ffn_norm, moe_group_w, moe_group_b, moe_expert_w, moe_expert_b,
              moe_w_gate, moe_w_up, moe_w_down):
    h = x
    k_sh = None
    v_sh = None
    for layer in range(DEPTH):
        if layer < N_A_LAYERS:
            i = layer
            h = h + forgetting_attention(rms_norm(h, a_norm[i]), a_w_in[i], a_b_f[i],
                                         a_q_gain[i], a_k_gain[i], a_w_out[i])
        else:
            if layer == N_A_LAYERS:
                k_sh, v_sh = shared_kv(h, kv_norm, kv_w, kv_k_gain, positions)
            j = layer - N_A_LAYERS
            h = h + dilated_attention(rms_norm(h, b_norm[j]), positions, k_sh, v_sh,
                                      b_w_q[j], b_q_gain[j], b_w_out[j])
        h = h + hier_moe(rms_norm(h, ffn_norm[layer]), moe_group_w[layer], moe_group_b[layer],
                         moe_expert_w[layer], moe_expert_b[layer],
                         moe_w_gate[layer], moe_w_up[layer], moe_w_down[layer])
    return h
```

```python
import numpy as np
import ml_dtypes
from contextlib import ExitStack

import concourse.bass as bass
import concourse.mybir as mybir
from concourse.bass_utils import run_bass_kernel_spmd

F32 = mybir.dt.float32
BF16 = mybir.dt.bfloat16
I32 = mybir.dt.int32
AF = mybir.ActivationFunctionType
ALU = mybir.AluOpType
AX = mybir.AxisListType

NCORES = 8
D = 1024
SEQ = 8192
TOK = 2048
EPS = 1e-6
NEXP = 16
FF = 256


class Tok:
    __slots__ = ("w", "r", "rd", "excl")

    def __init__(self, excl=False):
        self.w = None
        self.r = {}
        self.rd = []
        self.excl = excl


class Op:
    __slots__ = ("eng", "fn", "waits", "signal", "val", "dma", "dsem", "dval")


ENGS = ("pe", "act", "dve", "pool", "sp")
NRING = {"sp": 12, "pool": 8}


class Prog:
    def __init__(self, nc, stack):
        self.nc = nc
        self.stack = stack
        self.ops = {e: [] for e in ENGS}
        self.sem = {e: stack.enter_context(nc.semaphore("pg_" + e)) for e in ENGS if e != "sp"}
        self.ring = {q: [stack.enter_context(nc.semaphore("rg_%s%d" % (q, i))) for i in range(n)]
                     for q, n in NRING.items()}
        self.ring_val = {q: [0] * n for q, n in NRING.items()}
        self.ring_last = {q: [None] * n for q, n in NRING.items()}
        self.ring_next = {q: 0 for q in NRING}
        self.nbuf = 0
        self.out_dmas = []

    def sb(self, shape, dtype, name=None):
        self.nbuf += 1
        return self.stack.enter_context(self.nc.sbuf_tensor("%s_%d" % (name or "sb", self.nbuf), list(shape), dtype))

    def ps(self, name=None):
        self.nbuf += 1
        return self.stack.enter_context(self.nc.psum_tensor("%s_%d" % (name or "ps", self.nbuf), [128, 512], F32))

    def op(self, eng, fn, reads=(), writes=(), dma=False):
        o = Op()
        o.eng = eng
        o.fn = fn
        o.signal = False
        o.val = 0
        o.dma = dma
        o.dsem = None
        o.dval = 0
        deps = set()
        for t in reads:
            if t.w is not None:
                deps.add(t.w)
            if t.excl:
                deps.update(o2 for e2, o2 in t.r.items() if e2 != eng)
        for t in writes:
            if t.w is not None:
                deps.add(t.w)
            deps.update(t.r.values())
            deps.update(t.rd)
        if dma:
            q = eng
            i = self.ring_next[q]
            self.ring_next[q] = (i + 1) % NRING[q]
            prev = self.ring_last[q][i]
            if prev is not None:
                deps.add(prev)
            self.ring_val[q][i] += 16
            o.dsem = self.ring[q][i]
            o.dval = self.ring_val[q][i]
            self.ring_last[q][i] = o
        waits = []
        for d in deps:
            if d.dma:
                waits.append(d)
            elif d.eng == eng and eng == "pe":
                continue
            else:
                d.signal = True
                waits.append(d)
        o.waits = waits
        for t in reads:
            if dma:
                t.rd.append(o)
            else:
                t.r[eng] = o
        for t in writes:
            t.w = o
            t.r = {}
            t.rd = []
        self.ops[eng].append(o)
        return o

    def finalize(self):
        for e in ENGS:
            c = 0
            for o in self.ops[e]:
                if o.signal:
                    c += 1
                o.val = c

    def emit_engine(self, eng, e):
        waited = {}
        for o in self.ops[eng]:
            need = {}
            for d in o.waits:
                if d.dma:
                    s, v = d.dsem, d.dval
                else:
                    s, v = self.sem[d.eng], d.val
                k = id(s)
                if waited.get(k, 0) >= v:
                    continue
                if k not in need or need[k][1] < v:
                    need[k] = (s, v)
            for k, (s, v) in need.items():
                e.wait_ge(s, v)
                waited[k] = v
            ins = o.fn(e)
            if o.dma:
                ins.then_inc(o.dsem, 16)
            elif o.signal:
                ins.then_inc(self.sem[eng], 1)
        if eng == "sp":
            for o in self.out_dmas:
                e.wait_ge(o.dsem, o.dval)

    def run(self):
        self.finalize()
        nc = self.nc
        with nc.Block() as block:
            block.sync(lambda e: self.emit_engine("sp", e))
            block.scalar(lambda e: self.emit_engine("act", e))
            block.vector(lambda e: self.emit_engine("dve", e))
            block.gpsimd(lambda e: self.emit_engine("pool", e))
            block.tensor(lambda e: self.emit_engine("pe", e))

    def dma(self, out, in_, reads=(), writes=(), q="sp", is_out=False, **kw):
        o = self.op(q, lambda e: e.dma_start(out=out, in_=in_, **kw), reads, writes, dma=True)
        if is_out:
            self.out_dmas.append(o)
        return o

    def mm(self, out, lhsT, rhs, start, stop, reads=(), writes=()):
        return self.op("pe", lambda e: e.matmul(out, lhsT, rhs, start=start, stop=stop), reads, writes)

    def transpose(self, out, in_, ident, reads=(), writes=()):
        return self.op("pe", lambda e: e.transpose(out, in_, ident), reads, writes)

    def act(self, out, in_, func, reads=(), writes=(), **kw):
        return self.op("act", lambda e: e.activation(out, in_, func, **kw), reads, writes)


class Ring:
    def __init__(self, items, excl=False):
        self.items = [(b, Tok(excl)) for b in items]
        self.i = 0

    def next(self):
        it = self.items[self.i]
        self.i = (self.i + 1) % len(self.items)
        return it


def new_nc():
    return bass.Bass("TRN2", target_bir_lowering=False)


def dram_in(nc, name, shape, dt):
    return nc.dram_tensor(name, list(shape), dt, kind="ExternalInput").ap()


def dram_out(nc, name, shape, dt):
    return nc.dram_tensor(name, list(shape), dt, kind="ExternalOutput").ap()


def load_weight_bf16(P, dst, dst_tok, src, kchunks, ncols, colstep=1024, rows=128):
    if not hasattr(P, "wstage"):
        P.wstage = Ring([P.sb([128, 1024], F32, "wst") for _ in range(3)])
    for kc in range(kchunks):
        for c0 in range(0, ncols, colstep):
            c1 = min(ncols, c0 + colstep)
            stg, stt = P.wstage.next()
            P.dma(stg[0:rows, 0:c1 - c0], src[kc * rows:(kc + 1) * rows, c0:c1], writes=[stt])
            P.op("pool", lambda e, stg=stg, kc=kc, c0=c0, c1=c1: e.tensor_copy(dst[0:rows, kc, c0:c1], stg[0:rows, 0:c1 - c0]),
                 reads=[stt], writes=[dst_tok])


def rms_rows(P, C, src_blocks, nb, gain_bc, gain_tok, xn_ring, consume):
    ss = C["ss"]
    for b, (ap, tk) in enumerate(src_blocks):
        junk, jt = C["junk"].next()
        P.act(junk[:], ap, AF.Square, reads=[tk], writes=[jt, C["ss_tok"]], accum_out=ss[:, b:b + 1])
    P.act(C["lnv"][:, 0:nb], ss[:, 0:nb], AF.Ln, reads=[C["ss_tok"], C["eps_tok"]], writes=[C["lnv_tok"]],
          scale=1.0 / D, bias=C["eps_col"][:, 0:1])
    P.act(C["rstd"][:, 0:nb], C["lnv"][:, 0:nb], AF.Exp, reads=[C["lnv_tok"]], writes=[C["rstd_tok"]], scale=-0.5)
    for b, (ap, tk) in enumerate(src_blocks):
        xn, xt = xn_ring.next()
        rs = C["rstd"][:, b:b + 1]
        P.op("dve", lambda e, xn=xn, ap=ap, rs=rs: e.scalar_tensor_tensor(
            out=xn[:], in0=ap, scalar=rs, in1=gain_bc[:], op0=ALU.mult, op1=ALU.mult),
            reads=[tk, C["rstd_tok"], gain_tok], writes=[xt])
        consume(b, xn, xt)


def norm_consts(P, nc_):
    C = {}
    C["ss"] = P.sb([128, 16], F32, "ss")
    C["ss_tok"] = Tok()
    C["lnv"] = P.sb([128, 16], F32, "lnv")
    C["lnv_tok"] = Tok()
    C["rstd"] = P.sb([128, 16], F32, "rstd")
    C["rstd_tok"] = Tok()
    C["junk"] = Ring([P.sb([128, 1024], BF16, "junk") for _ in range(2)])
    C["eps_col"] = P.sb([128, 1], F32, "epsc")
    C["eps_tok"] = Tok()
    P.op("dve", lambda e: e.memset(C["eps_col"][:], EPS), writes=[C["eps_tok"]])
    C["one_col"] = P.sb([128, 1], F32, "onec")
    P.op("dve", lambda e: e.memset(C["one_col"][:], 1.0), writes=[C["eps_tok"]])
    return C


TWO_PI_HI = 6.28125
TWO_PI_LO = 0.0019353071795864769
PI_LO = 3.1415925
MAGIC = 12582912.0


def build_pre(kind):
    nc = new_nc()
    A = kind == "A"
    ncols = {"A": 3 * D + 16, "KV": 2 * D, "BQ": 3 * D}[kind]
    wcols = {"A": 3 * D, "KV": 2 * D, "BQ": 3 * D}[kind]
    h = dram_in(nc, "h", [TOK, D], F32)
    gnorm = dram_in(nc, "gnorm", [D], F32)
    w_in = dram_in(nc, "w", [D, ncols], F32)
    gains = dram_in(nc, "gains", [128, 4], F32)
    identd = dram_in(nc, "ident", [128, 128], F32)
    if A:
        b_f = dram_in(nc, "b_f", [16], F32)
        QT = dram_out(nc, "QT", [16, 64, TOK], BF16)
        KT = dram_out(nc, "KT", [16, 64, TOK], BF16)
        V = dram_out(nc, "V", [TOK, 16, 65], BF16)
        LF = dram_out(nc, "LF", [16, TOK], F32)
        chunks = [(c * 128, 0, QT[2 * c:2 * c + 2].rearrange("h d t -> (h d) t")) for c in range(8)] + \
                 [(D + c * 128, 1, KT[2 * c:2 * c + 2].rearrange("h d t -> (h d) t")) for c in range(8)]
        vcol0, grp = 2 * D, 64
    else:
        pos = dram_in(nc, "pos", [TOK], I32)
        invf = dram_in(nc, "invf", [128, 1], F32)
        rotd = dram_in(nc, "rot", [128, 128], F32)
        grp = 128
        if kind == "KV":
            KT = dram_out(nc, "KT", [8, 128, TOK], BF16)
            V = dram_out(nc, "V", [TOK, 8, 128], BF16)
            chunks = [(c * 128, 0, KT[c]) for c in range(8)]
            vcol0 = D
        else:
            QT = dram_out(nc, "QT", [24, 128, TOK], BF16)
            chunks = [(c * 128, c // 8, QT[c]) for c in range(24)]
            vcol0 = None

    with ExitStack() as st, nc.allow_low_precision("bf16 matmul operands by design"):
        P = Prog(nc, st)
        C = norm_consts(P, nc)
        psr = Ring([P.ps("ps") for _ in range(8)], excl=True)
        w = P.sb([128, 8, wcols], BF16, "w")
        w_tok = Tok()
        gain_bc = P.sb([128, D], F32, "gbc")
        gain_tok = Tok()
        ident = P.sb([128, 128], F32, "ident")
        ident_tok = Tok()
        gcol = P.sb([128, 4], F32, "gcol")
        gcol_tok = Tok()
        ones_blk = P.sb([128, 128], BF16, "onesblk")
        ones_tok = Tok()
        P.dma(ident[:], identd, writes=[ident_tok])
        P.dma(gain_bc[:], gnorm.partition_broadcast(128), writes=[gain_tok])
        P.dma(gcol[:], gains, writes=[gcol_tok])
        if A:
            P.op("dve", lambda e: e.tensor_scalar(gcol[:, 0:1], gcol[:, 0:1], 0.125, None, ALU.mult),
                 reads=[gcol_tok], writes=[gcol_tok])
        if grp == 64:
            P.op("pool", lambda e: e.memset(ones_blk[:], 0.0), writes=[ones_tok])
            P.op("pool", lambda e: e.memset(ones_blk[0:64, 0:64], 1.0), writes=[ones_tok])
            P.op("pool", lambda e: e.memset(ones_blk[64:128, 64:128], 1.0), writes=[ones_tok])
        else:
            P.op("pool", lambda e: e.memset(ones_blk[:], 1.0), writes=[ones_tok])
        if A:
            wf = P.sb([128, 8, 16], F32, "wf")
            wf_tok = Tok()
            nbf = P.sb([16, 1], F32, "nbf")
            nbf_tok = Tok()
            P.dma(nbf[:], b_f.rearrange("(p o) -> p o", o=1), writes=[nbf_tok])
            P.op("dve", lambda e: e.tensor_scalar(nbf[:], nbf[:], -1.0, None, ALU.mult), reads=[nbf_tok], writes=[nbf_tok])
            P.dma(wf[:], w_in[:, 3 * D:3 * D + 16].rearrange("(k p) c -> p k c", p=128), writes=[wf_tok])
            f1_ring = Ring([P.sb([16, 512], F32, "f1") for _ in range(2)])
            f2_ring = Ring([P.sb([16, 512], F32, "f2") for _ in range(2)])
        else:
            rot = P.sb([128, 128], F32, "rot")
            rot_tok = Tok()
            P.dma(rot[:], rotd, writes=[rot_tok])
            ivf = P.sb([128, 1], F32, "ivf")
            hp = P.sb([128, 1], F32, "hpi")
            P.op("dve", lambda e: e.memset(hp[:], float(np.pi / 2)), writes=[C["eps_tok"]])
            posi = P.sb([128, TOK], I32, "posi")
            ang = P.sb([128, TOK], F32, "ang")
            kk = P.sb([128, TOK], F32, "kk")
            cosT = P.sb([128, TOK], F32, "cosT")
            sinT = P.sb([128, TOK], F32, "sinT")
            tt = Tok()
            cs_tok = Tok()
            P.dma(ivf[:], invf, writes=[tt])
            P.dma(posi[:], pos.partition_broadcast(128), writes=[tt])
            dvr = lambda fn: P.op("dve", fn, reads=[tt], writes=[tt])
            dvr(lambda e: e.tensor_copy(ang[:], posi[:]))
            dvr(lambda e: e.tensor_scalar(ang[:], ang[:], ivf[:, 0:1], None, ALU.mult))
            dvr(lambda e: e.tensor_scalar(kk[:], ang[:], float(1.0 / (2 * np.pi)), None, ALU.mult))
            dvr(lambda e: e.tensor_scalar(kk[:], kk[:], MAGIC, None, ALU.add))
            dvr(lambda e: e.tensor_scalar(kk[:], kk[:], MAGIC, None, ALU.subtract))
            dvr(lambda e: e.scalar_tensor_tensor(out=ang[:], in0=kk[:], scalar=-TWO_PI_HI, in1=ang[:], op0=ALU.mult, op1=ALU.add))
            dvr(lambda e: e.scalar_tensor_tensor(out=ang[:], in0=kk[:], scalar=-TWO_PI_LO, in1=ang[:], op0=ALU.mult, op1=ALU.add))
            dvr(lambda e: e.tensor_scalar(kk[:], ang[:], float(np.pi), float(-2 * np.pi), ALU.is_gt, ALU.mult))
            dvr(lambda e: e.tensor_tensor(ang[:], ang[:], kk[:], ALU.add))
            dvr(lambda e: e.tensor_scalar(kk[:], ang[:], float(-np.pi), float(2 * np.pi), ALU.is_lt, ALU.mult))
            dvr(lambda e: e.tensor_tensor(ang[:], ang[:], kk[:], ALU.add))
            dvr(lambda e: e.tensor_scalar(ang[:], ang[:], PI_LO, -PI_LO, ALU.min, ALU.max))
            P.act(sinT[:], ang[:], AF.Sin, reads=[tt], writes=[cs_tok])
            dvr(lambda e: e.tensor_scalar(kk[:], ang[:], -1.0, None, ALU.mult))
            dvr(lambda e: e.tensor_tensor(kk[:], kk[:], ang[:], ALU.max))
            P.act(cosT[:], kk[:], AF.Sin, reads=[tt, C["eps_tok"]], writes=[cs_tok], scale=-1.0, bias=hp[:, 0:1])
            xr_ring = Ring([P.sb([128, 512], F32, "xr") for _ in range(2)])
            t2_ring = Ring([P.sb([128, 512], F32, "t2") for _ in range(2)])
        load_weight_bf16(P, w, w_tok, w_in, 8, wcols)

        hring = Ring([P.sb([128, 4, D], F32, "ht") for _ in range(2)])
        xnring = Ring([P.sb([128, D], F32, "xn") for _ in range(4)])
        xnT = P.sb([128, 8, 512], BF16, "xnT")
        xnT_tok = Tok()
        raw_ring = Ring([P.sb([128, 512], F32, "raw") for _ in range(2)])
        sq_ring = Ring([P.sb([128, 512], BF16, "sq") for _ in range(2)])
        ln_ring = Ring([P.sb([128, 512], F32, "ln") for _ in range(2)])
        rs_ring = Ring([P.sb([128, 512], F32, "rs") for _ in range(2)])
        ob_ring = Ring([P.sb([128, 512], BF16, "ob") for _ in range(3)])
        if A:
            xnTf = P.sb([128, 8, 512], F32, "xnTf")
            xnTf_tok = Tok()
            va_ring = Ring([P.sb([128, 16, 65], BF16, "va") for _ in range(2)])
            for va, vt in va_ring.items:
                P.op("pool", lambda e, va=va: e.memset(va[:, :, 64:65], 1.0), writes=[vt])
        elif vcol0 is not None:
            va_ring = Ring([P.sb([128, D], BF16, "va") for _ in range(2)])

        for t in range(4):
            tcs = slice(t * 512, (t + 1) * 512)
            ht, ht_tok = hring.next()
            P.dma(ht[:], h[tcs, :].rearrange("(b p) d -> p b d", p=128), writes=[ht_tok])
            xns = []
            rms_rows(P, C, [(ht[:, b, :], ht_tok) for b in range(4)], 4, gain_bc, gain_tok, xnring,
                     lambda b, xn, xt: xns.append((xn, xt)))
            for kc in range(8):
                tp, tpt = psr.next()
                for b in range(4):
                    xn, xt = xns[b]
                    P.transpose(tp[:, b * 128:(b + 1) * 128], xn[:, kc * 128:(kc + 1) * 128], ident[:],
                                reads=[xt, ident_tok], writes=[tpt])
                P.act(xnT[:, kc, :], tp[:], AF.Copy, reads=[tpt], writes=[xnT_tok])
                if A:
                    P.op("dve", lambda e, tp=tp, kc=kc: e.tensor_copy(xnTf[:, kc, :], tp[:]), reads=[tpt], writes=[xnTf_tok])
            if A:
                pf, pft = psr.next()
                for kc in range(8):
                    P.mm(pf[0:16, :], wf[:, kc, :], xnTf[:, kc, :], kc == 0, kc == 7, reads=[wf_tok, xnTf_tok], writes=[pft])
                f1, f1t = f1_ring.next()
                f2, f2t = f2_ring.next()
                P.act(f1[:], pf[0:16, :], AF.Exp, reads=[pft, nbf_tok], writes=[f1t], scale=-1.0, bias=nbf[:, 0:1])
                P.act(f2[:], f1[:], AF.Ln, reads=[f1t, C["eps_tok"]], writes=[f2t], bias=C["one_col"][0:16, 0:1])
                P.op("dve", lambda e, f1=f1, f2=f2: e.tensor_scalar(f1[:], f2[:], -1.0, None, ALU.mult), reads=[f2t], writes=[f1t])
                P.dma(LF[:, tcs], f1[:], reads=[f1t], is_out=True)
            for (col0, gi, dst) in chunks:
                pq, pqt = psr.next()
                for kc in range(8):
                    P.mm(pq[:], w[:, kc, col0:col0 + 128], xnT[:, kc, :], kc == 0, kc == 7, reads=[w_tok, xnT_tok], writes=[pqt])
                raw, rawt = raw_ring.next()
                P.act(raw[:], pq[:], AF.Copy, reads=[pqt], writes=[rawt])
                sq, sqt = sq_ring.next()
                P.op("dve", lambda e, sq=sq, raw=raw: e.tensor_tensor(sq[:], raw[:], raw[:], ALU.mult), reads=[rawt], writes=[sqt])
                pss, psst = psr.next()
                P.mm(pss[:], ones_blk[:], sq[:], True, True, reads=[ones_tok, sqt], writes=[psst])
                ln, lnt = ln_ring.next()
                P.act(ln[:], pss[:], AF.Ln, reads=[psst, C["eps_tok"]], writes=[lnt], scale=1.0 / grp, bias=C["eps_col"][:, 0:1])
                rs, rst = rs_ring.next()
                P.act(rs[:], ln[:], AF.Exp, reads=[lnt], writes=[rst], scale=-0.5)
                ob, obt = ob_ring.next()
                if A:
                    P.op("dve", lambda e, ob=ob, raw=raw, rs=rs, gi=gi: e.scalar_tensor_tensor(
                        out=ob[:], in0=raw[:], scalar=gcol[:, gi:gi + 1], in1=rs[:], op0=ALU.mult, op1=ALU.mult),
                        reads=[rawt, rst, gcol_tok], writes=[obt])
                else:
                    xr, xrt = xr_ring.next()
                    P.op("dve", lambda e, xr=xr, raw=raw, rs=rs, gi=gi: e.scalar_tensor_tensor(
                        out=xr[:], in0=raw[:], scalar=gcol[:, gi:gi + 1], in1=rs[:], op0=ALU.mult, op1=ALU.mult),
                        reads=[rawt, rst, gcol_tok], writes=[xrt])
                    pr, prt = psr.next()
                    P.mm(pr[:], rot[:], xr[:], True, True, reads=[rot_tok, xrt], writes=[prt])
                    t2, t2t = t2_ring.next()
                    P.op("dve", lambda e, t2=t2, pr=pr, tcs=tcs: e.tensor_tensor(t2[:], pr[:], sinT[:, tcs], ALU.mult),
                         reads=[prt, cs_tok], writes=[t2t])
                    P.op("dve", lambda e, xr=xr, tcs=tcs: e.tensor_tensor(xr[:], xr[:], cosT[:, tcs], ALU.mult),
                         reads=[xrt, cs_tok], writes=[xrt])
                    P.op("dve", lambda e, ob=ob, xr=xr, t2=t2: e.tensor_tensor(ob[:], xr[:], t2[:], ALU.add),
                         reads=[xrt, t2t], writes=[obt])
                P.dma(dst[:, tcs], ob[:], reads=[obt], is_out=True)
            if vcol0 is not None:
                for b in range(4):
                    va, vat = va_ring.next()
                    for half in range(2):
                        pv, pvt = psr.next()
                        for kc in range(8):
                            P.mm(pv[:], xnT[:, kc, b * 128:(b + 1) * 128], w[:, kc, vcol0 + half * 512:vcol0 + (half + 1) * 512],
                                 kc == 0, kc == 7, reads=[w_tok, xnT_tok], writes=[pvt])
                        if A:
                            P.op("dve", lambda e, va=va, pv=pv, half=half: e.tensor_copy(
                                va[:, half * 8:(half + 1) * 8, 0:64], pv[:].rearrange("p (h d) -> p h d", d=64)),
                                reads=[pvt], writes=[vat])
                        else:
                            P.op("dve", lambda e, va=va, pv=pv, half=half: e.tensor_copy(va[:, half * 512:(half + 1) * 512], pv[:]),
                                 reads=[pvt], writes=[vat])
                    blk = t * 4 + b
                    if A:
                        P.dma(V[blk * 128:(blk + 1) * 128, :, :], va[:], reads=[vat], is_out=True)
                    else:
                        P.dma(V[blk * 128:(blk + 1) * 128, :, :].rearrange("p h d -> p (h d)"), va[:], reads=[vat], is_out=True)
        P.run()
    return nc


BIG = 1.0e30


def build_k3(ntok=TOK, nexp=NEXP):
    nc = new_nc()
    NB = ntok // 128
    NT = ntok // 512
    h = dram_in(nc, "h", [ntok, D], F32)
    OTd = dram_in(nc, "OT", [D, ntok], BF16)
    w_out = dram_in(nc, "w_out", [D, D], F32)
    fnorm = dram_in(nc, "fnorm", [D], F32)
    wr = dram_in(nc, "wr", [D, 20], F32)
    br = dram_in(nc, "br", [20], F32)
    wg = dram_in(nc, "wg", [nexp, D, FF], F32)
    wu = dram_in(nc, "wu", [nexp, D, FF], F32)
    wd = dram_in(nc, "wd", [nexp, FF, D], F32)
    identd = dram_in(nc, "ident", [128, 128], F32)
    hout = dram_out(nc, "hout", [ntok, D], F32)

    with ExitStack() as st, nc.allow_low_precision("bf16 matmul operands by design"):
        P = Prog(nc, st)
        C = norm_consts(P, nc)
        psr = Ring([P.ps("ps") for _ in range(8)], excl=True)
        hs = P.sb([128, NB, D], F32, "h")
        h_toks = [Tok() for _ in range(NB)]
        xnT = P.sb([128, 8, ntok], BF16, "xnT")
        xnT_toks = [Tok() for _ in range(NT)]
        R = P.sb([128, 16384], BF16, "R")
        R_tok = Tok()
        gain_bc = P.sb([128, D], F32, "gbc")
        gain_tok = Tok()
        ident = P.sb([128, 128], F32, "ident")
        ident_tok = Tok()
        wr_sb = P.sb([128, 8, 20], F32, "wr")
        wr_tok = Tok()
        br_bc = P.sb([128, 20], F32, "brbc")
        br_tok = Tok()
        comb = P.sb([128, NB, 16], F32, "comb")
        comb_toks = [Tok() for _ in range(NB)]
        P.wstage = Ring([P.sb([128, 1024], F32, "wst") for _ in range(2)])

        P.dma(ident[:], identd, writes=[ident_tok])
        P.dma(gain_bc[:], fnorm.partition_broadcast(128), writes=[gain_tok])
        P.dma(wr_sb[:], wr.rearrange("(k p) c -> p k c", p=128), writes=[wr_tok])
        P.dma(br_bc[:], br.partition_broadcast(128), writes=[br_tok])
        for b in range(NB):
            P.dma(hs[:, b, :], h[b * 128:(b + 1) * 128, :], writes=[h_toks[b]])

        hb = NB // 2
        OT = R[:, 0:8 * hb * 128].rearrange("p (k t) -> p k t", k=8)
        WO = R[:, 8192:16384].rearrange("p (k c) -> p k c", k=8)
        wo_tok = Tok()
        load_weight_bf16(P, WO, wo_tok, w_out, 8, D)
        for hf in range(2):
            for kc in range(8):
                P.dma(OT[:, kc, :], OTd[kc * 128:(kc + 1) * 128, hf * hb * 128:(hf + 1) * hb * 128], writes=[R_tok])
            for bb in range(hb):
                b = hf * hb + bb
                for half in range(2):
                    ps, pst = psr.next()
                    for kc in range(8):
                        P.mm(ps[:], OT[:, kc, bb * 128:(bb + 1) * 128], WO[:, kc, half * 512:(half + 1) * 512],
                             kc == 0, kc == 7, reads=[R_tok, wo_tok], writes=[pst])
                    hv = hs[:, b, half * 512:(half + 1) * 512]
                    P.op("dve", lambda e, hv=hv, ps=ps: e.tensor_tensor(hv, ps[:], hv, ALU.add),
                         reads=[pst], writes=[h_toks[b]])

        xnring = Ring([P.sb([128, D], F32, "xn") for _ in range(4)])
        xnTf = P.sb([128, 8, 512], F32, "xnTf")
        xnTf_tok = Tok()
        lg = P.sb([128, 20], F32, "lg")
        sm = P.sb([128, 96], F32, "sm")
        smt = Tok()

        def dv(fn, reads=(), writes=()):
            return P.op("dve", fn, list(reads) + [smt], list(writes) + [smt])

        for t in range(NT):
            xns = []
            rms_rows(P, C, [(hs[:, t * 4 + b, :], h_toks[t * 4 + b]) for b in range(4)], 4, gain_bc, gain_tok, xnring,
                     lambda b, xn, xt: xns.append((xn, xt)))
            for kc in range(8):
                tp, tpt = psr.next()
                for b in range(4):
                    xn, xt = xns[b]
                    P.transpose(tp[:, b * 128:(b + 1) * 128], xn[:, kc * 128:(kc + 1) * 128], ident[:],
                                reads=[xt, ident_tok], writes=[tpt])
                P.act(xnT[:, kc, t * 512:(t + 1) * 512], tp[:], AF.Copy, reads=[tpt], writes=[xnT_toks[t]])
                P.op("dve", lambda e, tp=tp, kc=kc: e.tensor_copy(xnTf[:, kc, :], tp[:]), reads=[tpt], writes=[xnTf_tok])
            for b in range(4):
                blk = t * 4 + b
                pl, plt = psr.next()
                for kc in range(8):
                    P.mm(pl[:, 0:20], xnTf[:, kc, b * 128:(b + 1) * 128], wr_sb[:, kc, :], kc == 0, kc == 7,
                         reads=[xnTf_tok, wr_tok], writes=[plt])
                L = lg
                dv(lambda e, pl=pl: e.tensor_tensor(lg[:], pl[:, 0:20], br_bc[:], ALU.add), reads=[plt, br_tok])
                c = lambda i, n=1: sm[:, i:i + n]
                dv(lambda e: e.tensor_reduce(c(0), lg[:, 0:4], AX.X, ALU.max))
                dv(lambda e: e.tensor_scalar(c(10, 4), lg[:, 0:4], c(0), None, ALU.subtract))
                P.act(c(10, 4), c(10, 4), AF.Exp, reads=[smt], writes=[smt], accum_out=c(1))
                dv(lambda e: e.reciprocal(c(2), c(1)))
                dv(lambda e: e.tensor_scalar(c(14, 4), lg[:, 0:4], c(0), None, ALU.is_equal))
                dv(lambda e: e.tensor_scalar(c(14, 4), c(14, 4), BIG, -BIG, ALU.mult, ALU.add))
                for g in range(4):
                    dv(lambda e, g=g: e.tensor_scalar(c(20 + 4 * g, 4), lg[:, 4 + 4 * g:8 + 4 * g], c(14 + g), None, ALU.add))
                dv(lambda e: e.tensor_reduce(c(3), c(20, 16), AX.X, ALU.max))
                dv(lambda e: e.tensor_scalar(c(36, 16), c(20, 16), c(3), None, ALU.is_equal))
                dv(lambda e: e.scalar_tensor_tensor(out=c(52, 16), in0=c(36, 16), scalar=-BIG, in1=c(20, 16),
                                                    op0=ALU.mult, op1=ALU.add))
                dv(lambda e: e.tensor_reduce(c(4), c(52, 16), AX.X, ALU.max))
                dv(lambda e: e.tensor_scalar(c(68, 16), c(52, 16), c(4), None, ALU.is_equal))
                dv(lambda e: e.tensor_tensor(c(5), c(4), c(3), ALU.subtract))
                P.act(c(6), c(5), AF.Exp, reads=[smt], writes=[smt])
                dv(lambda e: e.tensor_scalar(c(6), c(6), 1.0, None, ALU.add))
                dv(lambda e: e.reciprocal(c(6), c(6)))
                dv(lambda e: e.tensor_tensor(c(7), c(2), c(6), ALU.mult))
                dv(lambda e: e.tensor_tensor(c(8), c(2), c(7), ALU.subtract))
                dv(lambda e: e.tensor_scalar(c(36, 16), c(36, 16), c(7), None, ALU.mult))
                dv(lambda e, blk=blk: e.scalar_tensor_tensor(out=comb[:, blk, :], in0=c(68, 16), scalar=c(8), in1=c(36, 16),
                                                              op0=ALU.mult, op1=ALU.add), writes=[comb_toks[blk]])

        def ebuf(i):
            base = i * 6144
            return (R[:, base:base + 2048].rearrange("p (k c) -> p k c", k=8),
                    R[:, base + 2048:base + 4096].rearrange("p (k c) -> p k c", k=8),
                    R[:, base + 4096:base + 6144].rearrange("p (k c) -> p k c", k=2))
        ebufs = [ebuf(0), ebuf(1)]
        etoks = [Tok(), Tok()]
        sg_ring = Ring([P.sb([128, 512], F32, "sg") for _ in range(2)])
        hid_ring = Ring([P.sb([128, 2, 512], BF16, "hid") for _ in range(2)])
        first = True
        for ex in range(nexp):
            WG, WU, WD = ebufs[ex % 2]
            et = etoks[ex % 2]
            wtoks = [et, R_tok] if ex < 2 else [et]
            for (dst, src, kch, ncol) in ((WG, wg[ex], 8, FF), (WU, wu[ex], 8, FF)):
                for k0 in (0, 4):
                    stg, stt = P.wstage.next()
                    P.dma(stg[:].rearrange("p (k c) -> p k c", k=4), src[k0 * 128:(k0 + 4) * 128, :].rearrange("(k p) c -> p k c", p=128),
                          writes=[stt])
                    P.op("pool", lambda e, dst=dst, stg=stg, k0=k0: e.tensor_copy(dst[:, k0:k0 + 4, :], stg[:].rearrange("p (k c) -> p k c", k=4)),
                         reads=[stt], writes=wtoks)
            for fc in range(2):
                stg, stt = P.wstage.next()
                P.dma(stg[:], wd[ex, fc * 128:(fc + 1) * 128, :], writes=[stt])
                P.op("pool", lambda e, WD=WD, stg=stg, fc=fc: e.tensor_copy(WD[:, fc, :], stg[:]), reads=[stt], writes=wtoks)
            for t in range(NT):
                hid, hidt = hid_ring.next()
                for fc in range(2):
                    pg, pgt = psr.next()
                    for kc in range(8):
                        P.mm(pg[:], WG[:, kc, fc * 128:(fc + 1) * 128], xnT[:, kc, t * 512:(t + 1) * 512], kc == 0, kc == 7,
                             reads=[et, xnT_toks[t]], writes=[pgt])
                    sg, sgt = sg_ring.next()
                    P.act(sg[:], pg[:], AF.Silu, reads=[pgt], writes=[sgt])
                    pu, put = psr.next()
                    for kc in range(8):
                        P.mm(pu[:], WU[:, kc, fc * 128:(fc + 1) * 128], xnT[:, kc, t * 512:(t + 1) * 512], kc == 0, kc == 7,
                             reads=[et, xnT_toks[t]], writes=[put])
                    P.op("dve", lambda e, hid=hid, fc=fc, pu=pu, sg=sg: e.tensor_tensor(hid[:, fc, :], pu[:], sg[:], ALU.mult),
                         reads=[put, sgt], writes=[hidt])
                for b in range(4):
                    blk = t * 4 + b
                    for half in range(2):
                        py, pyt = psr.next()
                        for fc in range(2):
                            P.mm(py[:], hid[:, fc, b * 128:(b + 1) * 128], WD[:, fc, half * 512:(half + 1) * 512], fc == 0, fc == 1,
                                 reads=[hidt, et], writes=[pyt])
                        hv = hs[:, blk, half * 512:(half + 1) * 512]
                        P.op("dve", lambda e, hv=hv, py=py, blk=blk, ex=ex: e.scalar_tensor_tensor(
                            out=hv, in0=py[:], scalar=comb[:, blk, ex:ex + 1], in1=hv, op0=ALU.mult, op1=ALU.add),
                            reads=[pyt, comb_toks[blk]], writes=[h_toks[blk]])
        for b in range(NB):
            P.dma(hout[b * 128:(b + 1) * 128, :], hs[:, b, :], reads=[h_toks[b]], is_out=True)
        P.run()
    return nc


def build_k2(S=SEQ, NH=4):
    nc = new_nc()
    NCH = S // 512
    NKB = S // 128
    QT = dram_in(nc, "QT", [NH, 64, S], BF16)
    KT = dram_in(nc, "KT", [NH, 64, S], BF16)
    V = dram_in(nc, "V", [NH, 128, NKB, 65], BF16)
    LF = dram_in(nc, "LF", [NH, S], F32)
    trid = dram_in(nc, "tri", [128, 128], BF16)
    OTd = dram_out(nc, "OT", [NH, 64, S], BF16)
    CA = nc.dram_tensor("ca_scratch", [NH, 6, S], BF16).ap()

    with ExitStack() as st, nc.allow_low_precision("bf16 matmul operands by design"):
        P = Prog(nc, st)
        tri = P.sb([128, 128], BF16, "tri")
        tri_tok = Tok()
        P.dma(tri[:], trid, writes=[tri_tok])
        onesf = P.sb([128, 64], F32, "onesf")
        onesf_tok = Tok()
        P.op("dve", lambda e: e.memset(onesf[:], 1.0), writes=[onesf_tok])

        CW = min(S, 2048)
        lf = P.sb([NH, CW], F32, "lf")
        one = P.sb([NH, CW], F32, "one")
        cc = P.sb([NH, CW], F32, "cc")
        pcs = [P.sb([NH, CW], BF16, "pc%d" % i) for i in range(3)]
        ng = P.sb([NH, CW], BF16, "ng")
        carry = P.sb([NH, 1], F32, "carry")
        t1 = Tok()
        ca_tok = Tok()
        P.op("dve", lambda e: e.memset(one[:], 1.0), writes=[t1])
        P.op("dve", lambda e: e.memset(carry[:], 0.0), writes=[t1])
        for ci in range(S // CW):
            cs = slice(ci * CW, (ci + 1) * CW)
            P.dma(lf[:], LF[:, cs], writes=[t1])
            P.op("dve", lambda e: e.tensor_tensor_scan(cc[:], one[:], lf[:], carry[:, 0:1], ALU.mult, ALU.add), reads=[t1], writes=[t1])
            P.op("dve", lambda e: e.tensor_copy(carry[:], cc[:, CW - 1:CW]), reads=[t1], writes=[t1])
            for i in range(3):
                P.op("dve", lambda e, i=i: e.tensor_copy(pcs[i][:], cc[:]), reads=[t1], writes=[t1])
                if i < 2:
                    P.op("dve", lambda e, i=i: e.tensor_tensor(cc[:], cc[:], pcs[i][:], ALU.subtract), reads=[t1], writes=[t1])
                P.dma(CA[:, i, cs], pcs[i][:], reads=[t1], writes=[ca_tok])
                P.op("dve", lambda e, i=i: e.tensor_scalar(ng[:], pcs[i][:], -1.0, None, ALU.mult), reads=[t1], writes=[t1])
                P.dma(CA[:, 3 + i, cs], ng[:], reads=[t1], writes=[ca_tok])

        kt_ring = Ring([P.sb([128, S], BF16, "kt") for _ in range(2)])
        qt_ring = Ring([P.sb([128, S], BF16, "qt") for _ in range(2)])
        v_ring = Ring([P.sb([128, NKB, 65], BF16, "v") for _ in range(2)])
        for (b, t) in kt_ring.items + qt_ring.items:
            P.op("pool", lambda e, b=b: e.memset(b[64:70, :], 1.0), writes=[t])
        s_ring = Ring([P.ps("s") for _ in range(4)], excl=True)
        o_ring = Ring([P.ps("o") for _ in range(2)], excl=True)
        bc_ring = Ring([P.ps("bc") for _ in range(1)], excl=True)
        p_ring = Ring([P.sb([128, 512], BF16, "p") for _ in range(4)])
        rl_ring = Ring([P.sb([128, 512], F32, "rl") for _ in range(2)])
        bcs_ring = Ring([P.sb([64, 512], F32, "bcs") for _ in range(2)])
        ot_ring = Ring([P.sb([64, 512], BF16, "ot") for _ in range(2)])

        for hh in range(NH):
            kt, ktt = kt_ring.next()
            qt, qtt = qt_ring.next()
            vt, vtt = v_ring.next()
            P.dma(kt[0:64, :], KT[hh], writes=[ktt])
            P.dma(kt[67:70, :], CA[hh, 3:6, :], reads=[ca_tok], writes=[ktt])
            P.dma(qt[0:64, :], QT[hh], writes=[qtt])
            P.dma(qt[64:67, :], CA[hh, 0:3, :], reads=[ca_tok], writes=[qtt])
            P.dma(vt[:], V[hh], writes=[vtt])
            for m in range(NCH):
                nkb = 4 * m + 4
                o, ot_ = o_ring.next()
                for kb in range(nkb):
                    j = kb - 4 * m
                    q0 = max(j, 0) * 128
                    sp, spt = s_ring.next()
                    P.mm(sp[:, q0:512], kt[0:70, kb * 128:(kb + 1) * 128], qt[0:70, m * 512 + q0:(m + 1) * 512], True, True,
                         reads=[ktt, qtt], writes=[spt])
                    pt, ptt = p_ring.next()
                    P.act(pt[:, q0:512], sp[:, q0:512], AF.Exp, reads=[spt], writes=[ptt])
                    if j >= 0:
                        P.op("pool", lambda e, pt=pt, q0=q0: e.tensor_tensor(pt[:, q0:q0 + 128], pt[:, q0:q0 + 128], tri[:], ALU.mult),
                             reads=[ptt, tri_tok], writes=[ptt])
                    P.mm(o[0:65, q0:512], vt[:, kb, :], pt[:, q0:512], kb == 0, kb == nkb - 1, reads=[vtt, ptt], writes=[ot_])
                rl, rlt = rl_ring.next()
                P.op("dve", lambda e, rl=rl, o=o: e.reciprocal(rl[64:65, :], o[64:65, :]), reads=[ot_], writes=[rlt])
                bc, bct = bc_ring.next()
                P.mm(bc[0:64, :], onesf[64:65, 0:64], rl[64:65, :], True, True, reads=[rlt, onesf_tok], writes=[bct])
                bcs, bcst = bcs_ring.next()
                P.act(bcs[:], bc[0:64, :], AF.Copy, reads=[bct], writes=[bcst])
                ob, obt = ot_ring.next()
                P.op("dve", lambda e, ob=ob, o=o, bcs=bcs: e.tensor_tensor(ob[:], o[0:64, :], bcs[:], ALU.mult),
                     reads=[ot_, bcst], writes=[obt])
                P.dma(OTd[hh, :, m * 512:(m + 1) * 512], ob[:], reads=[obt], is_out=True)
        P.run()
    return nc


B_DIL = (1, 4, 16)


def build_k5b(S=SEQ, NP=2):
    nc = new_nc()
    NBK = S // 128
    QTp = dram_in(nc, "QTp", [NP, 3, 128, S], BF16)
    KTp = dram_in(nc, "KTp", [NP, 3, 128, S], BF16)
    Vp = dram_in(nc, "Vp", [NP, 3, 128, NBK, 128], BF16)
    maskd = dram_in(nc, "masks", [128, 256], BF16)
    OTd = dram_out(nc, "OT", [NP, 128, S], BF16)
    scale = float(128 ** -0.5)

    with ExitStack() as st, nc.allow_low_precision("bf16 matmul operands by design"):
        P = Prog(nc, st)
        masks = P.sb([128, 256], BF16, "masks")
        masks_tok = Tok()
        P.dma(masks[:], maskd, writes=[masks_tok])
        ones = P.sb([128, 128], BF16, "ones")
        ones_tok = Tok()
        P.op("pool", lambda e: e.memset(ones[:], 1.0), writes=[ones_tok])
        qt_ring = Ring([P.sb([128, S], BF16, "qt") for _ in range(2)])
        kt_ring = Ring([P.sb([128, S], BF16, "kt") for _ in range(2)])
        v_ring = Ring([P.sb([128, NBK, 128], BF16, "v") for _ in range(2)])
        oacc = P.sb([128, S], F32, "oacc")
        lacc = P.sb([128, S], F32, "lacc")
        NSEG = S // 2048
        acc_toks = [Tok() for _ in range(NSEG)]
        s_ring = Ring([P.ps("s") for _ in range(4)], excl=True)
        o_ring = Ring([P.ps("o") for _ in range(4)], excl=True)
        p_ring = Ring([P.sb([128, 256], BF16, "p") for _ in range(4)])
        ob_ring = Ring([P.sb([128, 2048], BF16, "ob") for _ in range(2)])

        for pr in range(NP):
            for g in range(3):
                d = B_DIL[g]
                L = S // d
                nbr = L // 128
                qt, qtt = qt_ring.next()
                kt, ktt = kt_ring.next()
                vt, vtt = v_ring.next()
                P.dma(qt[:], QTp[pr, g], writes=[qtt])
                P.dma(kt[:], KTp[pr, g], writes=[ktt])
                P.dma(vt[:], Vp[pr, g], writes=[vtt])
                for B in range(NBK):
                    rho, n = B // nbr, B % nbr
                    has_prev = n > 0
                    c0 = 0 if has_prev else 128
                    sp, spt = s_ring.next()
                    if has_prev:
                        P.mm(sp[:, 0:128], kt[:, (B - 1) * 128:B * 128], qt[:, B * 128:(B + 1) * 128], True, True,
                             reads=[ktt, qtt], writes=[spt])
                    P.mm(sp[:, 128:256], kt[:, B * 128:(B + 1) * 128], qt[:, B * 128:(B + 1) * 128], True, True,
                         reads=[ktt, qtt], writes=[spt])
                    pt, ptt = p_ring.next()
                    P.act(pt[:, c0:256], sp[:, c0:256], AF.Exp, reads=[spt], writes=[ptt], scale=scale)
                    P.op("pool", lambda e, pt=pt, c0=c0: e.tensor_tensor(pt[:, c0:256], pt[:, c0:256], masks[:, c0:256], ALU.mult),
                         reads=[ptt, masks_tok], writes=[ptt])
                    o, ot_ = o_ring.next()
                    if has_prev:
                        P.mm(o[:, 0:128], vt[:, B - 1, :], pt[:, 0:128], True, False, reads=[vtt, ptt], writes=[ot_])
                    P.mm(o[:, 0:128], vt[:, B, :], pt[:, 128:256], not has_prev, True, reads=[vtt, ptt], writes=[ot_])
                    if has_prev:
                        P.mm(o[:, 128:256], ones[:], pt[:, 0:128], True, False, reads=[ones_tok, ptt], writes=[ot_])
                    P.mm(o[:, 128:256], ones[:], pt[:, 128:256], not has_prev, True, reads=[ones_tok, ptt], writes=[ot_])
                    base = 128 * n * d + rho
                    seg = base // 2048
                    cols = slice(base, base + 127 * d + 1, d)
                    at = acc_toks[seg]
                    if g == 0:
                        P.op("dve", lambda e, o=o, cols=cols: e.tensor_copy(oacc[:, cols], o[:, 0:128]), reads=[ot_], writes=[at])
                        P.op("dve", lambda e, o=o, cols=cols: e.tensor_copy(lacc[:, cols], o[:, 128:256]), reads=[ot_], writes=[at])
                    else:
                        P.op("dve", lambda e, o=o, cols=cols: e.tensor_tensor(oacc[:, cols], o[:, 0:128], oacc[:, cols], ALU.add),
                             reads=[ot_], writes=[at])
                        P.op("dve", lambda e, o=o, cols=cols: e.tensor_tensor(lacc[:, cols], o[:, 128:256], lacc[:, cols], ALU.add),
                             reads=[ot_], writes=[at])
            for sg in range(NSEG):
                cs = slice(sg * 2048, (sg + 1) * 2048)
                at = acc_toks[sg]
                P.op("dve", lambda e, cs=cs: e.reciprocal(lacc[:, cs], lacc[:, cs]), reads=[at], writes=[at])
                ob, obt = ob_ring.next()
                P.op("dve", lambda e, ob=ob, cs=cs: e.tensor_tensor(ob[:], oacc[:, cs], lacc[:, cs], ALU.mult), reads=[at], writes=[obt])
                P.dma(OTd[pr, :, cs], ob[:], reads=[obt], is_out=True)
        P.run()
    return nc


_BF = ml_dtypes.bfloat16
_PROGS = {}


def _prog(name, fn):
    if name not in _PROGS:
        _PROGS[name] = fn()
    return _PROGS[name]


def _run(nc, in_maps):
    res = run_bass_kernel_spmd(nc, in_maps, core_ids=list(range(NCORES)))
    return res.results


def _consts():
    ident = np.eye(128, dtype=np.float32)
    rot = np.zeros((128, 128), np.float32)
    for m in range(64):
        rot[m + 64, m] = -1.0
        rot[m, m + 64] = 1.0
    invf = (10000.0 ** (-(np.arange(128) % 64).astype(np.float32) / np.float32(64))).astype(np.float32).reshape(128, 1)
    pi = np.arange(128)[:, None]
    fi = np.arange(128)[None, :]
    tri = (pi <= fi).astype(np.float32).astype(_BF)
    masks = np.concatenate([(pi >= fi), (pi <= fi)], axis=1).astype(np.float32).astype(_BF)
    return ident, rot, invf, tri, masks


def _perm(g):
    d = B_DIL[g]
    L = SEQ // d
    return (np.arange(L)[None, :] * d + np.arange(d)[:, None]).reshape(-1)


def _to_token_sharded(OT_b):
    return [np.ascontiguousarray(OT_b[c // 4][:, (c % 4) * TOK:(c % 4 + 1) * TOK]) for c in range(NCORES)]


def kernel(x, positions, a_norm, a_w_in, a_b_f, a_q_gain, a_k_gain, a_w_out,
           kv_norm, kv_w, kv_k_gain, b_norm, b_w_q, b_q_gain, b_w_out,
           ffn_norm, moe_group_w, moe_group_b, moe_expert_w, moe_expert_b,
           moe_w_gate, moe_w_up, moe_w_down):
    f = lambda a: np.ascontiguousarray(np.asarray(a, dtype=np.float32))
    x = f(x)
    positions = np.ascontiguousarray(np.asarray(positions, dtype=np.int32))
    ident, rot, invf, tri, masks = _consts()
    cores = range(NCORES)
    tsl = lambda c: slice((c % 4) * TOK, (c % 4 + 1) * TOK)
    h = [np.ascontiguousarray(x[c // 4, tsl(c)]) for c in cores]

    def post(layer, OT_own, w_out):
        nc3 = _prog("k3", build_k3)
        wr = np.ascontiguousarray(np.concatenate([f(moe_group_w[layer]), f(moe_expert_w[layer])], axis=1))
        br = np.ascontiguousarray(np.concatenate([f(moe_group_b[layer]), f(moe_expert_b[layer])]))
        ims = [{"h": h[c], "OT": OT_own[c], "w_out": f(w_out), "fnorm": f(ffn_norm[layer]), "wr": wr, "br": br,
                "wg": f(moe_w_gate[layer]), "wu": f(moe_w_up[layer]), "wd": f(moe_w_down[layer]), "ident": ident}
               for c in cores]
        res = _run(nc3, ims)
        return [np.asarray(res[c]["hout"], dtype=np.float32) for c in cores]

    for i in range(2):
        nc1 = _prog("preA", lambda: build_pre("A"))
        gains = np.zeros((128, 4), np.float32)
        gains[:, 0] = np.tile(f(a_q_gain[i]), 2)
        gains[:, 1] = np.tile(f(a_k_gain[i]), 2)
        ims = [{"h": h[c], "gnorm": f(a_norm[i]), "w": f(a_w_in[i]), "gains": gains, "ident": ident, "b_f": f(a_b_f[i])}
               for c in cores]
        r1 = _run(nc1, ims)
        ims2 = []
        per_b = []
        for b in range(2):
            cs = [4 * b + r for r in range(4)]
            QTb = np.concatenate([np.asarray(r1[c]["QT"]) for c in cs], axis=2)
            KTb = np.concatenate([np.asarray(r1[c]["KT"]) for c in cs], axis=2)
            Vb = np.concatenate([np.asarray(r1[c]["V"]) for c in cs], axis=0)
            LFb = np.concatenate([np.asarray(r1[c]["LF"]) for c in cs], axis=1)
            per_b.append((QTb, KTb, Vb, LFb))
        for c in cores:
            QTb, KTb, Vb, LFb = per_b[c // 4]
            hs = slice(4 * (c % 4), 4 * (c % 4) + 4)
            Vh = np.ascontiguousarray(Vb[:, hs, :].reshape(SEQ // 128, 128, 4, 65).transpose(2, 1, 0, 3))
            ims2.append({"QT": np.ascontiguousarray(QTb[hs]), "KT": np.ascontiguousarray(KTb[hs]), "V": Vh,
                         "LF": np.ascontiguousarray(LFb[hs]), "tri": tri})
        nc2 = _prog("k2", build_k2)
        r2 = _run(nc2, ims2)
        OT_b = [np.concatenate([np.asarray(r2[4 * b + r]["OT"]) for r in range(4)], axis=0).reshape(D, SEQ) for b in range(2)]
        h = post(i, _to_token_sharded(OT_b), a_w_out[i])

    nc4 = _prog("preKV", lambda: build_pre("KV"))
    gains = np.zeros((128, 4), np.float32)
    gains[:, 0] = f(kv_k_gain)
    ims = [{"h": h[c], "gnorm": f(kv_norm), "w": f(kv_w), "gains": gains, "ident": ident,
            "pos": np.ascontiguousarray(positions[c // 4, tsl(c)]), "invf": invf, "rot": rot} for c in cores]
    r4 = _run(nc4, ims)
    perms = [_perm(g) for g in range(3)]
    KTp_c, Vp_c = [], []
    Kb = [np.concatenate([np.asarray(r4[4 * b + r]["KT"]) for r in range(4)], axis=2) for b in range(2)]
    Vb = [np.concatenate([np.asarray(r4[4 * b + r]["V"]) for r in range(4)], axis=0) for b in range(2)]
    for c in cores:
        b = c // 4
        kt = np.empty((2, 3, 128, SEQ), _BF)
        vp = np.empty((2, 3, 128, SEQ // 128, 128), _BF)
        for pi_ in range(2):
            hd = 2 * (c % 4) + pi_
            for g in range(3):
                kt[pi_, g] = Kb[b][hd][:, perms[g]]
                vp[pi_, g] = Vb[b][perms[g], hd, :].reshape(SEQ // 128, 128, 128).transpose(1, 0, 2)
        KTp_c.append(kt)
        Vp_c.append(vp)

    for j in range(2):
        nc5 = _prog("preBQ", lambda: build_pre("BQ"))
        gains = np.zeros((128, 4), np.float32)
        gains[:, 0:3] = f(b_q_gain[j]).T
        ims = [{"h": h[c], "gnorm": f(b_norm[j]), "w": f(b_w_q[j]), "gains": gains, "ident": ident,
                "pos": np.ascontiguousarray(positions[c // 4, tsl(c)]), "invf": invf, "rot": rot} for c in cores]
        r5 = _run(nc5, ims)
        Qb = [np.concatenate([np.asarray(r5[4 * b + r]["QT"]) for r in range(4)], axis=2) for b in range(2)]
        ims6 = []
        for c in cores:
            b = c // 4
            qp = np.empty((2, 3, 128, SEQ), _BF)
            for pi_ in range(2):
                hd = 2 * (c % 4) + pi_
                for g in range(3):
                    qp[pi_, g] = Qb[b][g * 8 + hd][:, perms[g]]
            ims6.append({"QTp": qp, "KTp": KTp_c[c], "Vp": Vp_c[c], "masks": masks})
        nc6 = _prog("k5b", build_k5b)
        r6 = _run(nc6, ims6)
        OT_b = [np.concatenate([np.asarray(r6[4 * b + r]["OT"]) for r in range(4)], axis=0).reshape(D, SEQ) for b in range(2)]
        h = post(2 + j, _to_token_sharded(OT_b), b_w_out[j])

    out = np.empty((2, SEQ, D), np.float32)
    for c in cores:
        out[c // 4, tsl(c)] = h[c]
    return out
```
